# Optimizing a Trainium2 kernel written in Bass

```python
import jax, jax.numpy as jnp
from jax import lax
import numpy as np

D_MODEL = 1024
BATCH = 8
SEQ = 4096
DEPTH = 4

RET_HEADS = 4
RET_HEAD_DIM = 128
RET_WIDTH = RET_HEADS * RET_HEAD_DIM
POOL_WINDOWS = (2, 4, 8, 16)
POOL_GROUPS = len(POOL_WINDOWS)
POOL_WIDTH = D_MODEL - RET_WIDTH
POOL_GROUP_CH = POOL_WIDTH // POOL_GROUPS
POOL_MAX_W = max(POOL_WINDOWS)
MIX_WIDTH = RET_WIDTH + POOL_WIDTH
IN_WIDTH = 4 * RET_WIDTH + POOL_WIDTH
CHUNK = 128
ROPE_BASE = 10000.0
D_FF = ((8 * D_MODEL // 3 + 255) // 256) * 256
PLE_DIM = 256
EPS = 1e-6

kernel_name = "hybrid_retention_pool_swiglu_ple"


def rmsnorm(x, g):
    xf = x.astype(jnp.float32)
    y = xf * lax.rsqrt(jnp.mean(xf * xf, axis=-1, keepdims=True) + EPS)
    return (y * g.astype(jnp.float32)).astype(x.dtype)


def rotary(x, positions):
    half = RET_HEAD_DIM // 2
    inv = 1.0 / (ROPE_BASE ** (jnp.arange(half, dtype=jnp.float32) * (2.0 / RET_HEAD_DIM)))
    ang = positions.astype(jnp.float32)[..., None] * inv
    cos = jnp.cos(ang)[:, :, None, :]
    sin = jnp.sin(ang)[:, :, None, :]
    xf = x.astype(jnp.float32)
    x1, x2 = xf[..., :half], xf[..., half:]
    return jnp.concatenate([x1 * cos - x2 * sin, x2 * cos + x1 * sin], axis=-1)


def retention(q, k, v, g):
    B, S, H, Dh = q.shape
    N = S // CHUNK
    log_gamma = jnp.log1p(-(2.0 ** (-5.0 - jnp.arange(H, dtype=jnp.float32))))
    qc = (q * (Dh ** -0.5)).reshape(B, N, CHUNK, H, Dh)
    kc = k.reshape(B, N, CHUNK, H, Dh)
    vc = v.astype(jnp.float32).reshape(B, N, CHUNK, H, Dh)
    idx = jnp.arange(CHUNK, dtype=jnp.float32)
    rel = idx[:, None] - idx[None, :]
    decay_in = jnp.where(rel[None] >= 0,
                         jnp.exp(jnp.maximum(rel, 0.0)[None] * log_gamma[:, None, None]), 0.0)
    scores = jnp.einsum('bnqhd,bnkhd->bnhqk', qc, kc) * decay_in[None, None]
    inner = jnp.einsum('bnhqk,bnkhd->bnqhd', scores, vc)
    zeta = jnp.exp((CHUNK - 1.0 - idx)[:, None] * log_gamma[None, :])
    kv = jnp.einsum('bnkhd,bnkhe->nbhde', kc * zeta[None, None, :, :, None], vc)
    chunk_decay = jnp.exp(CHUNK * log_gamma)[None, :, None, None]

    def step(state, kv_i):
        return state * chunk_decay + kv_i, state

    _, state_prev = lax.scan(step, jnp.zeros((B, H, Dh, Dh), jnp.float32), kv)
    xi = jnp.exp((idx + 1.0)[:, None] * log_gamma[None, :])
    cross = jnp.einsum('bnqhd,nbhde->bnqhe', qc * xi[None, None, :, :, None], state_prev)
    y = (inner + cross).reshape(B, S, H, Dh)
    y = y * lax.rsqrt(jnp.mean(y * y, axis=-1, keepdims=True) + EPS)
    y = y.reshape(B, S, H * Dh)
    return (jax.nn.silu(g.astype(jnp.float32)) * y).astype(g.dtype)


def pool_mixer(u, w_grp, scale):
    B, S, _ = u.shape
    uf = u.astype(jnp.float32).reshape(B, S, POOL_GROUPS, POOL_GROUP_CH)
    cs = jnp.cumsum(uf, axis=1)
    csp = jnp.pad(cs, ((0, 0), (POOL_MAX_W, 0), (0, 0), (0, 0)))
    t = jnp.arange(S)
    means = []
    for gi, w in enumerate(POOL_WINDOWS):
        win = csp[:, POOL_MAX_W:, gi] - csp[:, POOL_MAX_W - w:POOL_MAX_W - w + S, gi]
        cnt = jnp.minimum(t + 1, w).astype(jnp.float32)[None, :, None]
        means.append(win / cnt)
    pooled = jnp.stack(means, axis=2) - uf
    y = jnp.einsum('bsgc,gcd->bsgd', pooled, w_grp.astype(jnp.float32)).reshape(B, S, POOL_WIDTH)
    return (y * scale.astype(jnp.float32)).astype(u.dtype)


def setup_inputs(seed: int = 0) -> dict:
    key = jax.random.key(seed)
    ks = jax.random.split(key, 16)
    f32 = jnp.float32

    def nrm(k, shape, fan_in):
        return jax.random.normal(k, shape, f32) * (fan_in ** -0.5)

    def gain(k, shape):
        return 1.0 + 0.02 * jax.random.normal(k, shape, f32)

    x = jax.random.normal(ks[0], (BATCH, SEQ, D_MODEL), f32)
    p = jax.random.normal(ks[1], (DEPTH, BATCH, SEQ, PLE_DIM), f32)
    positions = jnp.broadcast_to(jnp.arange(SEQ, dtype=jnp.int32), (BATCH, SEQ))
    return {
        "x": x,
        "p": p,
        "positions": positions,
        "g_mix": gain(ks[2], (DEPTH, D_MODEL)),
        "w_in": nrm(ks[3], (DEPTH, D_MODEL, IN_WIDTH), D_MODEL),
        "pool_w": nrm(ks[4], (DEPTH, POOL_GROUPS, POOL_GROUP_CH, POOL_GROUP_CH), POOL_GROUP_CH),
        "pool_scale": 0.5 + 0.1 * jax.random.normal(ks[5], (DEPTH, POOL_WIDTH), f32),
        "w_out": nrm(ks[6], (DEPTH, MIX_WIDTH, D_MODEL), MIX_WIDTH),
        "g_ffn": gain(ks[7], (DEPTH, D_MODEL)),
        "w_gate": nrm(ks[8], (DEPTH, D_MODEL, D_FF), D_MODEL),
        "w_up": nrm(ks[9], (DEPTH, D_MODEL, D_FF), D_MODEL),
        "w_down": nrm(ks[10], (DEPTH, D_FF, D_MODEL), D_FF),
        "g_ple": gain(ks[11], (DEPTH, D_MODEL)),
        "w_ple_gate": nrm(ks[12], (DEPTH, D_MODEL, D_MODEL), D_MODEL),
        "w_ple_proj": nrm(ks[13], (DEPTH, PLE_DIM, D_MODEL), PLE_DIM),
        "g_final": gain(ks[14], (D_MODEL,)),
    }


def reference(x, p, positions, g_mix, w_in, pool_w, pool_scale, w_out, g_ffn,
              w_gate, w_up, w_down, g_ple, w_ple_gate, w_ple_proj, g_final):
    B, S, _ = x.shape
    for i in range(DEPTH):
        h = rmsnorm(x, g_mix[i])
        z = h @ w_in[i]
        q, k, v, g, u = jnp.split(z, [RET_WIDTH, 2 * RET_WIDTH, 3 * RET_WIDTH, 4 * RET_WIDTH], axis=-1)
        q = rotary(q.reshape(B, S, RET_HEADS, RET_HEAD_DIM), positions)
        k = rotary(k.reshape(B, S, RET_HEADS, RET_HEAD_DIM), positions)
        v = v.reshape(B, S, RET_HEADS, RET_HEAD_DIM)
        y_ret = retention(q, k, v, g)
        y_pool = pool_mixer(u, pool_w[i], pool_scale[i])
        mix = jnp.concatenate([y_ret, y_pool], axis=-1)
        x = x + (mix @ w_out[i]).astype(x.dtype)
        h2 = rmsnorm(x, g_ffn[i])
        ff = (jax.nn.silu(h2 @ w_gate[i]) * (h2 @ w_up[i])) @ w_down[i]
        x = x + ff.astype(x.dtype)
        gate = jax.nn.sigmoid((rmsnorm(x, g_ple[i]) @ w_ple_gate[i]).astype(jnp.float32))
        e = (p[i] @ w_ple_proj[i]).astype(jnp.float32)
        x = x + (gate * e).astype(x.dtype)
    return rmsnorm(x, g_final)
```

```python
import contextlib
import numpy as np
import ml_dtypes
import concourse.bass as bass
import concourse.mybir as mybir
from concourse.bass_utils import run_bass_kernel_spmd

F32 = mybir.dt.float32
BF16 = mybir.dt.bfloat16
I32 = mybir.dt.int32
ALU = mybir.AluOpType
AF = mybir.ActivationFunctionType

D = 1024
SEQ = 4096
DEPTH = 4
NH = 4
FF = 2816
NFB = FF // 128
PLE = 256
T = 1024
ST = 512
NS = T // ST
NCT = T // 128
EPS = 1e-6
RING = 4
SLOT = 4096
NTP = 8
NBT = 6
GAMMA = [1.0 - 2.0 ** (-5.0 - h) for h in range(NH)]
WINDOWS = (2, 4, 8, 16)
TWO_PI = 2.0 * np.pi
CW1 = 6.28125
CW2 = TWO_PI - CW1


def _merge(d, s):
    for k, v in s.items():
        if d.get(k, 0) < v:
            d[k] = v


class Buf:
    __slots__ = ("ap", "w", "r")

    def __init__(self, ap):
        self.ap = ap
        self.w = {}
        self.r = {}


class Sched:
    ENG = ("pe", "dve", "act", "pool", "sp")

    def __init__(self):
        self.streams = {e: [] for e in self.ENG}
        self.cnt = {e: 0 for e in self.ENG}
        self.seen = {e: {} for e in self.ENG}
        self.dma_cnt = {}

    def _wait(self, e, deps):
        for key, val in deps.items():
            if self.seen[e].get(key, 0) >= val:
                continue
            self.seen[e][key] = val
            self.streams[e].append(("wait", key, val))

    def _deps(self, reads, writes):
        deps = {}
        for b in reads:
            _merge(deps, b.w)
        for b in writes:
            _merge(deps, b.w)
            _merge(deps, b.r)
        return deps

    def _mark(self, key, val, reads, writes):
        for b in writes:
            b.w[key] = val
            b.r = {}
        for b in reads:
            if b in writes:
                continue
            if b.r.get(key, 0) < val:
                b.r[key] = val

    def op(self, e, fn, reads=(), writes=()):
        self._wait(e, self._deps(reads, writes))
        self.cnt[e] += 1
        self.streams[e].append(("op", fn, True))
        self._mark(e, self.cnt[e], reads, writes)

    def mm_group(self, fns, reads, writes):
        e = "pe"
        self._wait(e, self._deps(reads, writes))
        for fn in fns[:-1]:
            self.streams[e].append(("op", fn, False))
        self.cnt[e] += 1
        self.streams[e].append(("op", fns[-1], True))
        self._mark(e, self.cnt[e], reads, writes)

    def group(self, e, fns, reads=(), writes=()):
        self._wait(e, self._deps(reads, writes))
        for fn in fns[:-1]:
            self.streams[e].append(("op", fn, False))
        self.cnt[e] += 1
        self.streams[e].append(("op", fns[-1], True))
        self._mark(e, self.cnt[e], reads, writes)

    def dma(self, e, sem, fn, reads=(), writes=()):
        self._wait(e, self._deps(reads, writes))
        key = "dma:" + sem
        self.dma_cnt[key] = self.dma_cnt.get(key, 0) + 16
        self.streams[e].append(("dma", fn, key))
        self._mark(key, self.dma_cnt[key], reads, writes)

    def barrier(self, engines):
        for e in engines:
            deps = {o: self.cnt[o] for o in ("pe", "dve", "act", "pool") if o != e and self.cnt[o] > 0}
            self._wait(e, deps)

    def final_wait(self, e):
        self._wait(e, dict(self.dma_cnt))


def build_program(n_layers=DEPTH, n_tiles=SEQ // T, dbg=False):
    nc = bass.Bass("TRN2", target_bir_lowering=False)
    L = n_layers
    ntok = n_tiles * T
    dram = {}

    def din(name, shape, dt=F32):
        dram[name] = nc.dram_tensor(name, list(shape), dt, kind="ExternalInput").ap()
        return dram[name]

    xT = din("xT", [8, 128, SEQ])
    pT = din("pT", [DEPTH, 2, 128, SEQ])
    posr = din("posr", [128, SEQ], I32)
    w_in_d = din("w_in_r", [DEPTH, 5, 128, SLOT])
    w_out_d = din("w_out_r", [DEPTH, 2, 128, SLOT])
    w_gu_d = din("w_gu_r", [DEPTH, 11, 128, SLOT])
    w_dn_d = din("w_dn_r", [DEPTH, 8, 128, FF])
    w_pg_d = din("w_pg_r", [DEPTH, 2, 128, SLOT])
    w_pp_d = din("w_pp_r", [DEPTH, 128, 2048])
    w_pool_d = din("w_pool_r", [DEPTH, 128, 512])
    NSP = (DEPTH * 3 + 1) * 8 + DEPTH * 4 + 4
    smallp_d = din("smallp", [128, NSP])
    cbf_d = din("cbf", [128, 128 + 128 + 512 + 12 * 128], BF16)
    cf32_d = din("cf32", [128, 3 * 512])
    outT = nc.dram_tensor("outT", [8, 128, SEQ], F32, kind="ExternalOutput").ap()
    if dbg:
        dbgH = nc.dram_tensor("dbgH", [128, 8 * T], BF16, kind="ExternalOutput").ap()
        dbgQK = nc.dram_tensor("dbgQK", [128, 8 * T], BF16, kind="ExternalOutput").ap()

    S = Sched()
    es = contextlib.ExitStack()
    with es:
        def sb(name, shape, dt):
            return es.enter_context(nc.sbuf_tensor(name, list(shape), dt))

        Xt = sb("Xt", [128, 8, T], F32)
        Ht = sb("Ht", [128, 8, T], BF16)
        REG = sb("REG", [128, 22 * T], BF16)
        ring_t = sb("ring", [128, RING, SLOT], BF16)
        PTt = sb("PTt", [128, 2, T], BF16)
        poolw_t = sb("poolw", [128, 512], BF16)
        cbf = sb("cbf_s", [128, 128 + 128 + 512 + 12 * 128], BF16)
        cf32 = sb("cf32_s", [128, 3 * 512], F32)
        smallp = sb("smallp_s", [128, NSP], F32)
        C2t = sb("C2t", [128, T], F32)
        SSt = sb("SSt", [128, T], F32)
        POSI = sb("POSI", [128, ST], I32)
        wpp_t = sb("wpp", [128, 2048], BF16)
        RSTDt = sb("RSTDt", [128, 2, ST], F32)
        KI = sb("KI", [128, ST], I32)
        Ut = sb("Ut", [128, L, 512], F32)
        Sbf_t = sb("Sbf", [128, 2, 512], BF16)
        Rt = sb("Rt", [128, NCT, 512], BF16)
        Rc = sb("Rc", [128, L, 512], BF16)
        TPt = sb("TPt", [128, NTP, ST], F32)
        BTt = sb("BTt", [128, NBT, ST], BF16)
        print("sbuf bytes remaining/partition:", nc.sbuf_bytes_remaining)

        PSt = [es.enter_context(nc.psum_tensor("ps%d" % i, [128, ST], F32)) for i in range(7)]
        PSB = es.enter_context(nc.psum_tensor("psb", [128, 2, ST], BF16))

        X = [[Buf(Xt[:, k, s * ST:(s + 1) * ST]) for s in range(NS)] for k in range(8)]
        Hb = [[Buf(Ht[:, k, s * ST:(s + 1) * ST]) for s in range(NS)] for k in range(8)]
        oQ, oK, oV, oSG, oU = 0, 4 * T, 8 * T, 12 * T, 16 * T
        QT = [[Buf(REG[:, oQ + h * T + s * ST: oQ + h * T + (s + 1) * ST]) for s in range(NS)] for h in range(4)]
        KT = [[Buf(REG[:, oK + h * T + s * ST: oK + h * T + (s + 1) * ST]) for s in range(NS)] for h in range(4)]
        Vb = [Buf(REG[:, oV + c * 512: oV + (c + 1) * 512]) for c in range(NCT)]
        SG = [Buf(REG[:, oSG + s * 4 * ST: oSG + (s + 1) * 4 * ST]) for s in range(NS)]
        UT = [[Buf(REG[:, oU + g * T + s * ST: oU + g * T + (s + 1) * ST]) for s in range(NS)] for g in range(4)]
        ACTT = [[Buf(REG[:, f * T + s * ST: f * T + (s + 1) * ST]) for s in range(NS)] for f in range(NFB)]
        ring = [Buf(ring_t[:, i, :]) for i in range(RING)]
        PT = [[Buf(PTt[:, k, s * ST:(s + 1) * ST]) for s in range(NS)] for k in range(2)]
        PTall = [PT[k][s] for k in range(2) for s in range(NS)]
        POOLW = Buf(poolw_t[:])
        CB = Buf(cbf[:])
        CF = Buf(cf32[:])
        SP_ = Buf(smallp[:])
        ones_ap = cbf[:, 0:128]
        ident_ap = cbf[:, 128:256]
        mask_ap = cbf[:, 256:768]
        def m_ap(kind, g):
            o = 768 + (kind * 4 + g) * 128
            return cbf[:, o:o + 128]
        g128_ap = cf32[:, 0:512]
        vsc_ap = cf32[:, 512:1024]
        epsx_ap = cf32[:, 1024:1536]
        def gcol(idx, k):
            o = idx * 8 + k
            return smallp[:, o:o + 1]
        def psc(l, g):
            o = (DEPTH * 3 + 1) * 8 + l * 4 + g
            return smallp[:, o:o + 1]
        o_misc = (DEPTH * 3 + 1) * 8 + DEPTH * 4
        inv_ap = smallp[:, o_misc:o_misc + 1]
        phc_ap = smallp[:, o_misc + 1:o_misc + 2]
        phs_ap = smallp[:, o_misc + 2:o_misc + 3]
        C2 = [Buf(C2t[:, s * ST:(s + 1) * ST]) for s in range(NS)]
        SS = [Buf(SSt[:, s * ST:(s + 1) * ST]) for s in range(NS)]
        POSb = Buf(POSI[:])
        WPP = Buf(wpp_t[:])
        RSTD = [Buf(RSTDt[:, i, :]) for i in range(2)]
        KIb = Buf(KI[:])
        U = [Buf(Ut[:, l, :]) for l in range(L)]
        SBF = [Buf(Sbf_t[:, i, :]) for i in range(2)]
        R = [Buf(Rt[:, c, :]) for c in range(NCT)]
        RC = [Buf(Rc[:, l, :]) for l in range(L)]
        TP = [Buf(TPt[:, i, :]) for i in range(NTP)]
        BT = [Buf(BTt[:, i, :]) for i in range(NBT)]
        PB = [Buf(PSt[i][:]) for i in range(7)]
        PBB1 = Buf(PSB[:, 0, :])
        rot = {"tp": 0, "bt": 0, "pb": 0, "pbb": 0, "rs": 0}

        def tmp():
            rot["tp"] = (rot["tp"] + 1) % NTP
            return TP[rot["tp"]]

        def btmp():
            rot["bt"] = (rot["bt"] + 1) % NBT
            return BT[rot["bt"]]

        def bank():
            rot["pb"] = (rot["pb"] + 1) % 7
            return PB[rot["pb"]]

        def bbank():
            rot["pbb"] = (rot["pbb"] + 1) % 2
            return PBB[rot["pbb"]]

        loads = []
        for tt in range(n_tiles):
            for l in range(L):
                for g in range(5):
                    loads.append((w_in_d[l, g], SLOT))
                for g in range(2):
                    loads.append((w_out_d[l, g], SLOT))
                for g in range(11):
                    loads.append((w_gu_d[l, g], SLOT))
                for g in range(8):
                    loads.append((w_dn_d[l, g], FF))
                for g in range(2):
                    loads.append((w_pg_d[l, g], SLOT))
        wstate = {"issued": 0, "acq": 0, "rel": set(), "upto": 0}

        def try_issue():
            while wstate["issued"] < len(loads) and wstate["issued"] < wstate["upto"] + RING:
                i = wstate["issued"]
                src, n = loads[i]
                slot = ring[i % RING]
                dst = slot.ap[:, 0:n]
                S.dma("pool", "w%d" % (i % RING), lambda e, d=dst, s_=src: e.dma_start(out=d, in_=s_), writes=[slot])
                wstate["issued"] += 1

        def acquire():
            i = wstate["acq"]
            wstate["acq"] += 1
            assert i < wstate["upto"] + RING
            try_issue()
            assert i < wstate["issued"]
            return i, ring[i % RING]

        def release(i):
            wstate["rel"].add(i)
            while wstate["upto"] in wstate["rel"]:
                wstate["upto"] += 1
            try_issue()

        S.dma("sp", "c0", lambda e: e.dma_start(out=cbf[:], in_=cbf_d), writes=[CB])
        S.dma("sp", "c1", lambda e: e.dma_start(out=cf32[:], in_=cf32_d), writes=[CF])
        S.dma("sp", "c2", lambda e: e.dma_start(out=smallp[:], in_=smallp_d), writes=[SP_])
        for l in range(L):
            S.op("pool", lambda e, l=l: e.memset(Ut[:, l, :], 0.0), writes=[U[l]])
        try_issue()

        def mm(out_ap, lhsT_ap, rhs_ap, start, stop):
            return lambda e: e.matmul(out_ap, lhsT_ap, rhs_ap, start=start, stop=stop)

        def norm(s, gidx, dst, final_out=None):
            ssb = bank()
            for k in range(8):
                sq = btmp()
                S.op("act", lambda e, o=sq.ap, i=X[k][s].ap: e.activation(o, i, AF.Square),
                     reads=[X[k][s]], writes=[sq])
                S.mm_group([mm(ssb.ap, ones_ap, sq.ap, k == 0, k == 7)], reads=[sq, CB], writes=[ssb] if k == 0 else [])
                if k > 0:
                    ssb.w["pe"] = S.cnt["pe"]
            t = tmp()
            S.op("act", lambda e, o=t.ap, i=ssb.ap: e.activation(o, i, AF.Ln, bias=EPS, scale=1.0 / D),
                 reads=[ssb], writes=[t])
            rot["rs"] = (rot["rs"] + 1) % 2
            r = RSTD[rot["rs"]]
            S.op("act", lambda e, o=r.ap, i=t.ap: e.activation(o, i, AF.Exp, scale=-0.5),
                 reads=[t], writes=[r])
            for k in range(8):
                if final_out is None:
                    S.op("dve", lambda e, o=dst[k].ap, i=X[k][s].ap, g=gcol(gidx, k), rr=r.ap:
                         e.scalar_tensor_tensor(o, i, g, rr, ALU.mult, ALU.mult),
                         reads=[X[k][s], r, SP_], writes=[dst[k]])
                else:
                    ob = tmp()
                    S.op("dve", lambda e, o=ob.ap, i=X[k][s].ap, g=gcol(gidx, k), rr=r.ap:
                         e.scalar_tensor_tensor(o, i, g, rr, ALU.mult, ALU.mult),
                         reads=[X[k][s], r, SP_], writes=[ob])
                    final_out(k, ob)

        def proj_group(slot, lhs_of_k, rhs_bufs, nk):
            b = bank()
            fns = [mm(b.ap, lhs_of_k(k), rhs_bufs[k].ap, k == 0, k == nk - 1) for k in range(nk)]
            S.mm_group(fns, reads=[slot] + list(rhs_bufs), writes=[b])
            return b

        for tt in range(n_tiles):
            t0 = tt * T
            def load_x(tt_, s_):
                t0_ = tt_ * T
                for half in range(2):
                    ks = range(4 * half, 4 * half + 4)
                    bufs = [X[k][s_] for k in ks]
                    S.dma("sp", "x%d" % (s_ * 2 + half),
                          lambda e, half=half, s_=s_, t0_=t0_: e.dma_start(
                              out=Xt[:, 4 * half:4 * half + 4, s_ * ST:(s_ + 1) * ST],
                              in_=xT[4 * half:4 * half + 4, :, t0_ + s_ * ST:t0_ + (s_ + 1) * ST].rearrange("k p t -> p k t")),
                          writes=bufs)

            def build_tables(tt_):
                t0_ = tt_ * T
                for s in range(NS):
                    S.dma("sp", "pos", lambda e, s=s, t0_=t0_: e.dma_start(out=POSI[:], in_=posr[:, t0_ + s * ST:t0_ + (s + 1) * ST]), writes=[POSb])
                    a0 = tmp()
                    S.op("dve", lambda e, o=a0.ap: e.tensor_copy(o, POSI[:]), reads=[POSb], writes=[a0])
                    for (tab, ph) in ((C2[s], phc_ap), (SS[s], phs_ap)):
                        a1 = tmp()
                        S.op("dve", lambda e, o=a1.ap, i=a0.ap, ph=ph: e.tensor_scalar(o, i, inv_ap, ph, ALU.mult, ALU.add),
                             reads=[a0, SP_], writes=[a1])
                        kf = tmp()
                        S.op("dve", lambda e, o=kf.ap, i=a1.ap: e.tensor_scalar(o, i, 1.0 / TWO_PI, None, ALU.mult),
                             reads=[a1], writes=[kf])
                        S.op("dve", lambda e, i=kf.ap: e.tensor_copy(KI[:], i), reads=[kf], writes=[KIb])
                        kf2 = tmp()
                        S.op("dve", lambda e, o=kf2.ap: e.tensor_copy(o, KI[:]), reads=[KIb], writes=[kf2])
                        a2 = tmp()
                        S.op("dve", lambda e, o=a2.ap, k_=kf2.ap, a=a1.ap: e.scalar_tensor_tensor(o, k_, -CW1, a, ALU.mult, ALU.add),
                             reads=[kf2, a1], writes=[a2])
                        a3 = tmp()
                        S.op("dve", lambda e, o=a3.ap, k_=kf2.ap, a=a2.ap: e.scalar_tensor_tensor(o, k_, -CW2, a, ALU.mult, ALU.add),
                             reads=[kf2, a2], writes=[a3])
                        S.op("act", lambda e, o=tab.ap, i=a3.ap: e.activation(o, i, AF.Sin), reads=[a3], writes=[tab])

            if tt == 0:
                load_x(0, 0)
                load_x(0, 1)
                build_tables(0)
            has_next = (tt + 1 < n_tiles)

            for l in range(L):
                first_chunk_global = (tt == 0)
                S.barrier(["dve", "act", "pool"])
                S.dma("pool", "pt", lambda e, l=l, t0=t0: e.dma_start(out=PTt[:], in_=pT[l, :, :, t0:t0 + T].rearrange("k p t -> p k t")),
                      writes=PTall)
                S.dma("pool", "pw", lambda e, l=l: e.dma_start(out=poolw_t[:], in_=w_pool_d[l]), writes=[POOLW])
                S.dma("pool", "pp", lambda e, l=l: e.dma_start(out=wpp_t[:], in_=w_pp_d[l]), writes=[WPP])

                if l == 0:
                    norm(0, l * 3 + 0, [Hb[k][0] for k in range(8)])
                qk = [(QT, acquire()), (KT, acquire())]
                for s in range(NS):
                    if s == 1:
                        norm(1, l * 3 + 0, [Hb[k][1] for k in range(8)])
                    hs = [Hb[k][s] for k in range(8)]
                    for dstT, (wi_, slot) in qk:
                        for j in range(4):
                            b = proj_group(slot, lambda k, j=j, slot=slot: slot.ap[:, (j * 8 + k) * 128:(j * 8 + k + 1) * 128], hs, 8)
                            t1 = tmp()
                            S.op("dve", lambda e, o=t1.ap, i=b.ap, c=C2[s].ap: e.tensor_tensor(o, i, c, ALU.mult),
                                 reads=[b, C2[s]], writes=[t1])
                            t2 = tmp()
                            S.op("dve", lambda e, o=t2.ap, i=b.ap, c=SS[s].ap: e.tensor_tensor(o[0:64, :], i[64:128, :], c[64:128, :], ALU.mult),
                                 reads=[b, SS[s]], writes=[t2])
                            S.op("dve", lambda e, o=t2.ap, i=b.ap, c=SS[s].ap: e.tensor_tensor(o[64:128, :], i[0:64, :], c[0:64, :], ALU.mult),
                                 reads=[b, SS[s]], writes=[t2])
                            S.op("pool", lambda e, o=dstT[j][s].ap, a=t1.ap, bb=t2.ap: e.tensor_tensor(o, a, bb, ALU.add),
                                 reads=[t1, t2], writes=[dstT[j][s]])
                for dstT, (wi_, slot) in qk:
                    release(wi_)
                wi, slot = acquire()
                for s in range(NS):
                    for c in range(4):
                        hs = [Hb[k][s] for k in range(8)]
                        b = bank()
                        fns = [mm(b.ap, Hb[k][s].ap[:, c * 128:(c + 1) * 128], slot.ap[:, k * 512:(k + 1) * 512], k == 0, k == 7)
                               for k in range(8)]
                        S.mm_group(fns, reads=[slot] + hs, writes=[b])
                        vb = Vb[s * 4 + c]
                        S.op("dve", lambda e, o=vb.ap, i=b.ap: e.tensor_tensor(o, i, vsc_ap, ALU.mult),
                             reads=[b, CF], writes=[vb])
                release(wi)
                wi, slot = acquire()
                for s in range(NS):
                    hs = [Hb[k][s] for k in range(8)]
                    for j in range(4):
                        b = proj_group(slot, lambda k, j=j, slot=slot: slot.ap[:, (j * 8 + k) * 128:(j * 8 + k + 1) * 128], hs, 8)
                        o_ap = SG[s].ap.rearrange("p (c h q) -> p c h q", c=4, h=4)[:, :, j, :]
                        i_ap = b.ap.rearrange("p (c q) -> p c q", c=4)
                        S.op("act", lambda e, o=o_ap, i=i_ap: e.activation(o, i, AF.Silu), reads=[b], writes=[SG[s]])
                release(wi)
                wi, slot = acquire()
                for s in range(NS):
                    hs = [Hb[k][s] for k in range(8)]
                    for j in range(4):
                        b = proj_group(slot, lambda k, j=j, slot=slot: slot.ap[:, (j * 8 + k) * 128:(j * 8 + k + 1) * 128], hs, 8)
                        S.op("act", lambda e, o=UT[j][s].ap, i=b.ap: e.activation(o, i, AF.Copy), reads=[b], writes=[UT[j][s]])
                release(wi)

                sbi = 0
                S.op("dve", lambda e, u=U[l].ap: e.tensor_tensor(Sbf_t[:, 0, :], u, g128_ap, ALU.mult),
                     reads=[U[l], CF], writes=[SBF[0]])
                for s in range(NS):
                    mixr = [Hb[k][s] for k in range(4)]

                    def emit_scores(c):
                        cs = slice(c * 128, (c + 1) * 128)
                        sb_ = bank()
                        S.mm_group([mm(sb_.ap[:, h * 128:(h + 1) * 128], KT[h][s].ap[:, cs], QT[h][s].ap[:, cs], True, True)
                                    for h in range(4)], reads=[KT[h][s] for h in range(4)] + [QT[h][s] for h in range(4)], writes=[sb_])
                        pm = btmp()
                        S.op("dve", lambda e, o=pm.ap, i=sb_.ap: e.tensor_tensor(o, i, mask_ap, ALU.mult),
                             reads=[sb_, CB], writes=[pm])
                        return pm

                    def emit_transposes(c):
                        cs = slice(c * 128, (c + 1) * 128)
                        tb = PBB1
                        S.mm_group([lambda e, h=h, o=tb.ap, i=KT[h][s].ap[:, cs]: e.transpose(o[:, h * 128:(h + 1) * 128], i, ident_ap)
                                    for h in range(4)], reads=[KT[h][s] for h in range(4)] + [CB], writes=[tb])
                        ktok = btmp()
                        S.op("act", lambda e, o=ktok.ap, i=tb.ap: e.activation(o, i, AF.Copy), reads=[tb], writes=[ktok])
                        return ktok

                    for c in range(4):
                        cs = slice(c * 128, (c + 1) * 128)
                        rb = bank()
                        S.mm_group([mm(rb.ap[:, g * 128:(g + 1) * 128], UT[g][s].ap[:, cs], poolw_t[:, g * 128:(g + 1) * 128], True, True)
                                    for g in range(4)], reads=[UT[g][s] for g in range(4)] + [POOLW], writes=[rb])
                        rr = R[s * 4 + c]
                        S.op("act", lambda e, o=rr.ap, i=rb.ap: e.activation(o, i, AF.Copy), reads=[rb], writes=[rr])

                    def emit_pool2(g):
                        gc = slice(g * 128, (g + 1) * 128)
                        yb2 = bank()
                        fns = []
                        rds = [CB]
                        for c2 in range(4):
                            cs2 = slice(c2 * 128, (c2 + 1) * 128)
                            n = s * 4 + c2
                            cur = R[n]
                            rds.append(cur)
                            if first_chunk_global and n == 0:
                                fns.append(mm(yb2.ap[:, cs2], cur.ap[:, gc], m_ap(2, g), True, True))
                            else:
                                prev = R[n - 1] if n > 0 else RC[l]
                                rds.append(prev)
                                fns.append(mm(yb2.ap[:, cs2], cur.ap[:, gc], m_ap(0, g), True, False))
                                fns.append(mm(yb2.ap[:, cs2], prev.ap[:, gc], m_ap(1, g), False, True))
                        S.mm_group(fns, reads=rds, writes=[yb2])
                        S.op("act", lambda e, o=Hb[4 + g][s].ap, i=yb2.ap, sc=psc(l, g): e.activation(o, i, AF.Copy, scale=sc),
                             reads=[yb2, SP_], writes=[Hb[4 + g][s]])

                    pm_n = emit_scores(0)
                    ktok_n = emit_transposes(0)
                    for c in range(4):
                        cs = slice(c * 128, (c + 1) * 128)
                        vb = Vb[s * 4 + c]
                        pm, ktok = pm_n, ktok_n
                        sbf_cur = SBF[sbi]
                        sbf_ap = Sbf_t[:, sbi, :]
                        kvb = bank()
                        S.mm_group([mm(kvb.ap[:, h * 128:(h + 1) * 128], ktok.ap[:, h * 128:(h + 1) * 128], vb.ap[:, h * 128:(h + 1) * 128], True, True)
                                    for h in range(4)], reads=[ktok, vb], writes=[kvb])
                        S.group("dve", [lambda e, u=U[l].ap, kv=kvb.ap, hc=slice(h * 128, (h + 1) * 128), g=float(GAMMA[h] ** 128):
                                        e.scalar_tensor_tensor(u[:, hc], u[:, hc], g, kv[:, hc], ALU.mult, ALU.add) for h in range(4)],
                                reads=[kvb], writes=[U[l]])
                        last = (s == NS - 1 and c == 3)
                        if not last:
                            nxt = 1 - sbi
                            S.op("dve", lambda e, u=U[l].ap, nxt=nxt: e.tensor_tensor(Sbf_t[:, nxt, :], u, g128_ap, ALU.mult),
                                 reads=[U[l], CF], writes=[SBF[nxt]])
                        yb = bank()
                        fns = []
                        for h in range(4):
                            hc = slice(h * 128, (h + 1) * 128)
                            fns.append(mm(yb.ap[:, hc], vb.ap[:, hc], pm.ap[:, hc], True, False))
                            fns.append(mm(yb.ap[:, hc], sbf_ap[:, hc], QT[h][s].ap[:, cs], False, True))
                        S.mm_group(fns, reads=[vb, pm, sbf_cur] + [QT[h][s] for h in range(4)], writes=[yb])
                        sbi = 1 - sbi
                        emit_pool2(c)
                        if c < 3:
                            pm_n = emit_scores(c + 1)
                            ktok_n = emit_transposes(c + 1)
                        ysq = btmp()
                        S.op("act", lambda e, o=ysq.ap, i=yb.ap: e.activation(o, i, AF.Square), reads=[yb], writes=[ysq])
                        gsb = bank()
                        S.mm_group([mm(gsb.ap, ones_ap, ysq.ap, True, True)], reads=[ysq, CB], writes=[gsb])
                        tg = tmp()
                        S.op("dve", lambda e, o=tg.ap, i=gsb.ap: e.scalar_tensor_tensor(o, i, 1.0 / 128.0, epsx_ap, ALU.mult, ALU.add),
                             reads=[gsb, CF], writes=[tg])
                        rs = tmp()
                        S.op("act", lambda e, o=rs.ap, i=tg.ap: e.activation(o, i, AF.Ln), reads=[tg], writes=[rs])
                        S.op("act", lambda e, o=rs.ap: e.activation(o, o, AF.Exp, scale=-0.5), reads=[rs], writes=[rs])
                        rs2 = tmp()
                        S.op("pool", lambda e, o=rs2.ap, i=rs.ap, g=SG[s].ap[:, c * 512:(c + 1) * 512]: e.tensor_tensor(o, i, g, ALU.mult),
                             reads=[rs, SG[s]], writes=[rs2])
                        o_ap = Ht[:, 0:4, s * ST + c * 128: s * ST + (c + 1) * 128]
                        S.op("dve", lambda e, o=o_ap, y=yb.ap.rearrange("p (h q) -> p h q", h=4), r_=rs2.ap.rearrange("p (h q) -> p h q", h=4):
                             e.tensor_tensor(o, y, r_, ALU.mult), reads=[yb, rs2], writes=mixr)
                S.op("pool", lambda e, l=l: e.tensor_copy(Rc[:, l, :], Rt[:, NCT - 1, :]), reads=[R[NCT - 1]], writes=[RC[l]])

                if dbg and l == 0 and tt == 0:
                    allh = [Hb[k][s] for k in range(8) for s in range(NS)]
                    S.dma("sp", "dbg", lambda e: e.dma_start(out=dbgH, in_=Ht[:].rearrange("p k t -> p (k t)")), reads=allh)
                    allqk = [QT[h][s] for h in range(4) for s in range(NS)] + [KT[h][s] for h in range(4) for s in range(NS)]
                    S.dma("sp", "dbg", lambda e: e.dma_start(out=dbgQK, in_=REG[:, 0:8 * T]), reads=allqk)
                wslots = [acquire(), acquire()]

                def wout_block(s, jd):
                    wi_, slot = wslots[jd // 4]
                    j = jd % 4
                    hs = [Hb[k][s] for k in range(8)]
                    b = proj_group(slot, lambda k, j=j, slot=slot: slot.ap[:, (j * 8 + k) * 128:(j * 8 + k + 1) * 128], hs, 8)
                    S.op("dve", lambda e, x=X[jd][s].ap, i=b.ap: e.tensor_tensor(x, i, x, ALU.add),
                         reads=[b], writes=[X[jd][s]])

                for jd in range(8):
                    wout_block(0, jd)
                for jd in range(2):
                    wout_block(1, jd)
                norm(0, l * 3 + 1, [Hb[k][0] for k in range(8)])
                for jd in range(2, 8):
                    wout_block(1, jd)
                release(wslots[0][0])
                release(wslots[1][0])

                norm(1, l * 3 + 1, [Hb[k][1] for k in range(8)])
                S.barrier(["dve"])
                def gu_blocks(gi, slot, s):
                    hs = [Hb[k][s] for k in range(8)]
                    for jj in range(2):
                        f = gi * 2 + jj
                        base = jj * 2 * 8 * 128
                        ba = proj_group(slot, lambda k, base=base, slot=slot: slot.ap[:, base + k * 128: base + (k + 1) * 128], hs, 8)
                        bu = proj_group(slot, lambda k, base=base, slot=slot: slot.ap[:, base + (8 + k) * 128: base + (9 + k) * 128], hs, 8)
                        sg = tmp()
                        S.op("act", lambda e, o=sg.ap, i=ba.ap: e.activation(o, i, AF.Silu), reads=[ba], writes=[sg])
                        S.op("dve", lambda e, o=ACTT[f][s].ap, i=bu.ap, g=sg.ap: e.tensor_tensor(o, i, g, ALU.mult),
                             reads=[bu, sg], writes=[ACTT[f][s]])

                gi = 0
                while gi < 11:
                    grp = [gi] if gi == 10 else [gi, gi + 1]
                    held = [(g_, acquire()) for g_ in grp]
                    for s in range(NS):
                        for g_, (wi_, slot) in held:
                            gu_blocks(g_, slot, s)
                    for g_, (wi_, slot) in held:
                        release(wi_)
                    gi += len(grp)
                if l == L - 1 and has_next:
                    build_tables(tt + 1)
                def down_block(jd, slot, s):
                    acts = [ACTT[f][s] for f in range(NFB)]
                    b = proj_group(slot, lambda k, slot=slot: slot.ap[:, k * 128:(k + 1) * 128], acts, NFB)
                    S.op("dve", lambda e, x=X[jd][s].ap, i=b.ap: e.tensor_tensor(x, i, x, ALU.add),
                         reads=[b], writes=[X[jd][s]])

                for jd in range(5):
                    wi, slot = acquire()
                    for s in range(NS):
                        down_block(jd, slot, s)
                    release(wi)
                held = [(jd, acquire()) for jd in (5, 6, 7)]
                for jd, (wi_, slot) in held:
                    down_block(jd, slot, 0)
                down_block(5, held[0][1][1], 1)
                norm(0, l * 3 + 2, [Hb[k][0] for k in range(8)])
                for jd, (wi_, slot) in held[1:]:
                    down_block(jd, slot, 1)
                for jd, (wi_, slot) in held:
                    release(wi_)

                norm(1, l * 3 + 2, [Hb[k][1] for k in range(8)])
                wpp = WPP
                pslots = [acquire(), acquire()]

                def ple_block(s, jd):
                    wi_, slot = pslots[jd // 4]
                    j = jd % 4
                    hs = [Hb[k][s] for k in range(8)]
                    ba = proj_group(slot, lambda k, j=j, slot=slot: slot.ap[:, (j * 8 + k) * 128:(j * 8 + k + 1) * 128], hs, 8)
                    be = proj_group(wpp, lambda k, jd=jd, wpp=wpp: wpp.ap[:, (jd * 2 + k) * 128:(jd * 2 + k + 1) * 128],
                                    [PT[0][s], PT[1][s]], 2)
                    th = tmp()
                    S.op("act", lambda e, o=th.ap, i=ba.ap: e.activation(o, i, AF.Tanh, scale=0.5), reads=[ba], writes=[th])
                    tm = tmp()
                    S.op("dve", lambda e, o=tm.ap, t_=th.ap, i=be.ap: e.scalar_tensor_tensor(o, t_, 1.0, i, ALU.add, ALU.mult),
                         reads=[th, be], writes=[tm])
                    S.op("dve", lambda e, x=X[jd][s].ap, t_=tm.ap: e.scalar_tensor_tensor(x, t_, 0.5, x, ALU.mult, ALU.add),
                         reads=[tm], writes=[X[jd][s]])

                def store_fn(s, t0=t0):
                    def store(k, ob):
                        S.dma("sp", "o%d" % (k % 4),
                              lambda e, k=k, ob=ob, s=s, t0=t0: e.dma_start(out=outT[k, :, t0 + s * ST: t0 + (s + 1) * ST], in_=ob.ap),
                              reads=[ob])
                    return store

                for jd in range(8):
                    ple_block(0, jd)
                for jd in range(2):
                    ple_block(1, jd)
                if l < L - 1:
                    norm(0, (l + 1) * 3 + 0, [Hb[k][0] for k in range(8)])
                else:
                    norm(0, DEPTH * 3, None, final_out=store_fn(0))
                    if has_next:
                        load_x(tt + 1, 0)
                for jd in range(2, 8):
                    ple_block(1, jd)
                release(pslots[0][0])
                release(pslots[1][0])

            norm(1, DEPTH * 3, None, final_out=store_fn(1))
            if has_next:
                load_x(tt + 1, 1)

        S.final_wait("sp")

        sem_names = ["pe", "dve", "act", "pool"] + sorted(S.dma_cnt.keys())
        sems = {n: es.enter_context(nc.semaphore("s_" + n.replace(":", "_"))) for n in sem_names}
        block = es.enter_context(nc.Block())

        def replay(stream, ename):
            def run(eng):
                for item in stream:
                    if item[0] == "wait":
                        eng.wait_ge(sems[item[1]], item[2])
                    elif item[0] == "op":
                        ins = item[1](eng)
                        if item[2]:
                            ins.then_inc(sems[ename], 1)
                    else:
                        item[1](eng).then_inc(sems[item[2]], 16)
            return run

        block.tensor(replay(S.streams["pe"], "pe"))
        block.vector(replay(S.streams["dve"], "dve"))
        block.scalar(replay(S.streams["act"], "act"))
        block.gpsimd(replay(S.streams["pool"], "pool"))
        block.sync(replay(S.streams["sp"], "sp"))
    print("instr counts:", {e: len(v) for e, v in S.streams.items()})
    return nc


def _blk(w, ncol_blocks):
    K = w.shape[0] // 128
    return w.reshape(K, 128, ncol_blocks, 128).transpose(2, 1, 0, 3)


def _const_tables():
    bf = ml_dtypes.bfloat16
    ones = np.ones((128, 128), np.float32)
    ident = np.eye(128, dtype=np.float32)
    k = np.arange(128)[:, None]
    q = np.arange(128)[None, :]
    mask = (k <= q).astype(np.float32)
    mask4 = np.tile(mask, (1, 4))
    ms = []
    tp = np.arange(128)[:, None]
    t = np.arange(128)[None, :]
    for w in WINDOWS:
        m = ((t - tp >= 0) & (t - tp < w)).astype(np.float32) / w - (t == tp).astype(np.float32)
        ms.append(m)
    for w in WINDOWS:
        m = ((t + 128 - tp) < w).astype(np.float32) / w
        ms.append(m)
    for w in WINDOWS:
        cnt = np.minimum(t + 1, w).astype(np.float32)
        m = ((t - tp >= 0) & (t - tp < w)).astype(np.float32) / cnt - (t == tp).astype(np.float32)
        ms.append(m)
    cbf = np.concatenate([ones, ident, mask4] + ms, axis=1).astype(bf)
    g128 = np.zeros((128, 512), np.float32)
    vsc = np.zeros((128, 512), np.float32)
    epsx = np.zeros((128, 512), np.float32)
    p = np.arange(128, dtype=np.float64)
    for h in range(NH):
        g = GAMMA[h]
        g128[:, h * 128:(h + 1) * 128] = g ** 128
        vsc[:, h * 128:(h + 1) * 128] = (g ** (-(p + 1.0)))[:, None]
        epsx[:, h * 128:(h + 1) * 128] = (128.0 * EPS * g ** (-2.0 * (p + 1.0)))[None, :]
    cf32 = np.concatenate([g128, vsc, epsx], axis=1).astype(np.float32)
    return np.ascontiguousarray(cbf), np.ascontiguousarray(cf32)


def _prep_shared(g_mix, w_in, pool_w, pool_scale, w_out, g_ffn, w_gate, w_up, w_down, g_ple,
                 w_ple_gate, w_ple_proj, g_final):
    f = np.float32
    w_in_r = np.empty((DEPTH, 5, 128, SLOT), f)
    w_out_r = np.empty((DEPTH, 2, 128, SLOT), f)
    w_gu_r = np.empty((DEPTH, 11, 128, SLOT), f)
    w_dn_r = np.empty((DEPTH, 8, 128, FF), f)
    w_pg_r = np.empty((DEPTH, 2, 128, SLOT), f)
    w_pp_r = np.empty((DEPTH, 128, 2048), f)
    w_pool_r = np.empty((DEPTH, 128, 512), f)
    for l in range(DEPTH):
        wb = _blk(w_in[l], 20)
        for gi, gsrc in ((0, 0), (1, 1), (3, 3), (4, 4)):
            w_in_r[l, gi] = wb[gsrc * 4:(gsrc + 1) * 4].transpose(1, 0, 2, 3).reshape(128, SLOT)
        wv = w_in[l][:, 1024:1536].reshape(8, 128, 512).transpose(1, 0, 2)
        w_in_r[l, 2] = wv.reshape(128, SLOT)
        wo = _blk(w_out[l], 8)
        wp = _blk(w_ple_gate[l], 8)
        for gi in range(2):
            w_out_r[l, gi] = wo[gi * 4:(gi + 1) * 4].transpose(1, 0, 2, 3).reshape(128, SLOT)
            w_pg_r[l, gi] = wp[gi * 4:(gi + 1) * 4].transpose(1, 0, 2, 3).reshape(128, SLOT)
        wg = _blk(w_gate[l], NFB)
        wu = _blk(w_up[l], NFB)
        gu = np.stack([wg, wu], axis=1)
        gu = gu.reshape(11, 2, 2, 128, 8, 128).transpose(0, 3, 1, 2, 4, 5)
        w_gu_r[l] = gu.reshape(11, 128, SLOT)
        wd = _blk(w_down[l], 8)
        w_dn_r[l] = wd.reshape(8, 128, FF)
        pp = _blk(w_ple_proj[l], 8)
        w_pp_r[l] = pp.transpose(1, 0, 2, 3).reshape(128, 2048)
        w_pool_r[l] = pool_w[l].transpose(1, 0, 2).reshape(128, 512)
    NSP = (DEPTH * 3 + 1) * 8 + DEPTH * 4 + 4
    smallp = np.zeros((128, NSP), f)
    for l in range(DEPTH):
        for i, g in enumerate((g_mix[l], g_ffn[l], g_ple[l])):
            idx = l * 3 + i
            smallp[:, idx * 8:(idx + 1) * 8] = g.reshape(8, 128).T
        smallp[:, (DEPTH * 3 + 1) * 8 + l * 4:(DEPTH * 3 + 1) * 8 + (l + 1) * 4] = pool_scale[l].reshape(4, 128).T
    smallp[:, DEPTH * 3 * 8:(DEPTH * 3 + 1) * 8] = g_final.reshape(8, 128).T
    o = (DEPTH * 3 + 1) * 8 + DEPTH * 4
    half = 64
    inv = (np.float32(1.0) / (np.float32(10000.0) ** (np.arange(half, dtype=np.float32) * np.float32(2.0 / 128)))).astype(f)
    smallp[:, o] = np.concatenate([inv, inv])
    smallp[:, o + 1] = np.float32(np.pi / 2)
    smallp[:, o + 2] = np.concatenate([np.zeros(64, f), np.full(64, np.pi, f)])
    cbf, cf32 = _const_tables()
    return dict(w_in_r=w_in_r, w_out_r=w_out_r, w_gu_r=w_gu_r, w_dn_r=w_dn_r, w_pg_r=w_pg_r, w_pp_r=w_pp_r,
                w_pool_r=w_pool_r, smallp=smallp, cbf=cbf, cf32=cf32)


def run(inputs, n_layers=DEPTH, n_tiles=SEQ // T, cores=8, dbg=False):
    x = np.asarray(inputs["x"], np.float32)
    p = np.asarray(inputs["p"], np.float32)
    positions = np.asarray(inputs["positions"], np.int32)
    shared = _prep_shared(*[np.asarray(inputs[k], np.float32) for k in (
        "g_mix", "w_in", "pool_w", "pool_scale", "w_out", "g_ffn", "w_gate", "w_up", "w_down", "g_ple",
        "w_ple_gate", "w_ple_proj", "g_final")])
    nc = build_program(n_layers, n_tiles, dbg)
    in_maps = []
    for b in range(cores):
        m = dict(shared)
        m["xT"] = np.ascontiguousarray(x[b].T).reshape(8, 128, SEQ)
        m["pT"] = np.ascontiguousarray(p[:, b].transpose(0, 2, 1)).reshape(DEPTH, 2, 128, SEQ)
        m["posr"] = np.ascontiguousarray(np.broadcast_to(positions[b][None, :], (128, SEQ)))
        in_maps.append(m)
    res = run_bass_kernel_spmd(nc, in_maps, core_ids=list(range(cores)))
    outs = [np.ascontiguousarray(r["outT"].reshape(D, SEQ).T) for r in res.results]
    if dbg:
        return np.stack(outs, axis=0), res.results[0]
    return np.stack(outs, axis=0)


def kernel(**inputs):
    return run(inputs).astype(np.float32)
```

```python
import contextlib
import numpy as np
import ml_dtypes
import concourse.bass as bass
import concourse.mybir as mybir
from concourse.bass_utils import run_bass_kernel_spmd

F32 = mybir.dt.float32
BF16 = mybir.dt.bfloat16
I32 = mybir.dt.int32
ALU = mybir.AluOpType
AF = mybir.ActivationFunctionType

D = 1024
SEQ = 4096
DEPTH = 4
NH = 4
FF = 2816
NFB = FF // 128
PLE = 256
T = 1024
ST = 512
NS = T // ST
NCT = T // 128
EPS = 1e-6
RING = 4
SLOT = 4096
NTP = 8
NBT = 6
GAMMA = [1.0 - 2.0 ** (-5.0 - h) for h in range(NH)]
WINDOWS = (2, 4, 8, 16)
TWO_PI = 2.0 * np.pi
CW1 = 6.28125
CW2 = TWO_PI - CW1


def _merge(d, s):
    for k, v in s.items():
        if d.get(k, 0) < v:
            d[k] = v


class Buf:
    __slots__ = ("ap", "w", "r")

    def __init__(self, ap):
        self.ap = ap
        self.w = {}
        self.r = {}


class Sched:
    ENG = ("pe", "dve", "act", "pool", "sp")

    def __init__(self):
        self.streams = {e: [] for e in self.ENG}
        self.cnt = {e: 0 for e in self.ENG}
        self.seen = {e: {} for e in self.ENG}
        self.dma_cnt = {}

    def _wait(self, e, deps):
        for key, val in deps.items():
            if self.seen[e].get(key, 0) >= val:
                continue
            self.seen[e][key] = val
            self.streams[e].append(("wait", key, val))

    def _deps(self, reads, writes):
        deps = {}
        for b in reads:
            _merge(deps, b.w)
        for b in writes:
            _merge(deps, b.w)
            _merge(deps, b.r)
        return deps

    def _mark(self, key, val, reads, writes):
        for b in writes:
            b.w[key] = val
            b.r = {}
        for b in reads:
            if b in writes:
                continue
            if b.r.get(key, 0) < val:
                b.r[key] = val

    def op(self, e, fn, reads=(), writes=()):
        self._wait(e, self._deps(reads, writes))
        self.cnt[e] += 1
        self.streams[e].append(("op", fn, True))
        self._mark(e, self.cnt[e], reads, writes)

    def mm_group(self, fns, reads, writes):
        e = "pe"
        self._wait(e, self._deps(reads, writes))
        for fn in fns[:-1]:
            self.streams[e].append(("op", fn, False))
        self.cnt[e] += 1
        self.streams[e].append(("op", fns[-1], True))
        self._mark(e, self.cnt[e], reads, writes)

    def group(self, e, fns, reads=(), writes=()):
        self._wait(e, self._deps(reads, writes))
        for fn in fns[:-1]:
            self.streams[e].append(("op", fn, False))
        self.cnt[e] += 1
        self.streams[e].append(("op", fns[-1], True))
        self._mark(e, self.cnt[e], reads, writes)

    def dma(self, e, sem, fn, reads=(), writes=()):
        self._wait(e, self._deps(reads, writes))
        key = "dma:" + sem
        self.dma_cnt[key] = self.dma_cnt.get(key, 0) + 16
        self.streams[e].append(("dma", fn, key))
        self._mark(key, self.dma_cnt[key], reads, writes)

    def barrier(self, engines):
        for e in engines:
            deps = {o: self.cnt[o] for o in ("pe", "dve", "act", "pool") if o != e and self.cnt[o] > 0}
            self._wait(e, deps)

    def final_wait(self, e):
        self._wait(e, dict(self.dma_cnt))


def build_program(n_layers=DEPTH, n_tiles=SEQ // T, dbg=False):
    nc = bass.Bass("TRN2", target_bir_lowering=False)
    L = n_layers
    ntok = n_tiles * T
    dram = {}

    def din(name, shape, dt=F32):
        dram[name] = nc.dram_tensor(name, list(shape), dt, kind="ExternalInput").ap()
        return dram[name]

    xT = din("xT", [8, 128, SEQ])
    pT = din("pT", [DEPTH, 2, 128, SEQ])
    posr = din("posr", [128, SEQ], I32)
    w_in_d = din("w_in_r", [DEPTH, 5, 128, SLOT])
    w_out_d = din("w_out_r", [DEPTH, 2, 128, SLOT])
    w_gu_d = din("w_gu_r", [DEPTH, 11, 128, SLOT])
    w_dn_d = din("w_dn_r", [DEPTH, 8, 128, FF])
    w_pg_d = din("w_pg_r", [DEPTH, 2, 128, SLOT])
    w_pp_d = din("w_pp_r", [DEPTH, 128, 2048])
    w_pool_d = din("w_pool_r", [DEPTH, 128, 512])
    NSP = (DEPTH * 3 + 1) * 8 + DEPTH * 4 + 4
    smallp_d = din("smallp", [128, NSP])
    cbf_d = din("cbf", [128, 128 + 128 + 512 + 12 * 128], BF16)
    cf32_d = din("cf32", [128, 3 * 512])
    outT = nc.dram_tensor("outT", [8, 128, SEQ], F32, kind="ExternalOutput").ap()
    if dbg:
        dbgH = nc.dram_tensor("dbgH", [128, 8 * T], BF16, kind="ExternalOutput").ap()
        dbgQK = nc.dram_tensor("dbgQK", [128, 8 * T], BF16, kind="ExternalOutput").ap()

    S = Sched()
    es = contextlib.ExitStack()
    with es:
        def sb(name, shape, dt):
            return es.enter_context(nc.sbuf_tensor(name, list(shape), dt))

        Xt = sb("Xt", [128, 8, T], F32)
        Ht = sb("Ht", [128, 8, T], BF16)
        REG = sb("REG", [128, 22 * T], BF16)
        ring_t = sb("ring", [128, RING, SLOT], BF16)
        PTt = sb("PTt", [128, 2, T], BF16)
        poolw_t = sb("poolw", [128, 512], BF16)
        cbf = sb("cbf_s", [128, 128 + 128 + 512 + 12 * 128], BF16)
        cf32 = sb("cf32_s", [128, 3 * 512], F32)
        smallp = sb("smallp_s", [128, NSP], F32)
        C2t = sb("C2t", [128, T], F32)
        SSt = sb("SSt", [128, T], F32)
        POSI = sb("POSI", [128, ST], I32)
        wpp_t = sb("wpp", [128, 2048], BF16)
        RSTDt = sb("RSTDt", [128, 2, ST], F32)
        KI = sb("KI", [128, ST], I32)
        Ut = sb("Ut", [128, L, 512], F32)
        Sbf_t = sb("Sbf", [128, 2, 512], BF16)
        Rt = sb("Rt", [128, NCT, 512], BF16)
        Rc = sb("Rc", [128, L, 512], BF16)
        TPt = sb("TPt", [128, NTP, ST], F32)
        BTt = sb("BTt", [128, NBT, ST], BF16)
        print("sbuf bytes remaining/partition:", nc.sbuf_bytes_remaining)

        PSt = [es.enter_context(nc.psum_tensor("ps%d" % i, [128, ST], F32)) for i in range(7)]
        PSB = es.enter_context(nc.psum_tensor("psb", [128, 2, ST], BF16))

        X = [[Buf(Xt[:, k, s * ST:(s + 1) * ST]) for s in range(NS)] for k in range(8)]
        Hb = [[Buf(Ht[:, k, s * ST:(s + 1) * ST]) for s in range(NS)] for k in range(8)]
        oQ, oK, oV, oSG, oU = 0, 4 * T, 8 * T, 12 * T, 16 * T
        QT = [[Buf(REG[:, oQ + h * T + s * ST: oQ + h * T + (s + 1) * ST]) for s in range(NS)] for h in range(4)]
        KT = [[Buf(REG[:, oK + h * T + s * ST: oK + h * T + (s + 1) * ST]) for s in range(NS)] for h in range(4)]
        Vb = [Buf(REG[:, oV + c * 512: oV + (c + 1) * 512]) for c in range(NCT)]
        SG = [Buf(REG[:, oSG + s * 4 * ST: oSG + (s + 1) * 4 * ST]) for s in range(NS)]
        UT = [[Buf(REG[:, oU + g * T + s * ST: oU + g * T + (s + 1) * ST]) for s in range(NS)] for g in range(4)]
        ACTT = [[Buf(REG[:, f * T + s * ST: f * T + (s + 1) * ST]) for s in range(NS)] for f in range(NFB)]
        ring = [Buf(ring_t[:, i, :]) for i in range(RING)]
        PT = [[Buf(PTt[:, k, s * ST:(s + 1) * ST]) for s in range(NS)] for k in range(2)]
        PTall = [PT[k][s] for k in range(2) for s in range(NS)]
        POOLW = Buf(poolw_t[:])
        CB = Buf(cbf[:])
        CF = Buf(cf32[:])
        SP_ = Buf(smallp[:])
        ones_ap = cbf[:, 0:128]
        ident_ap = cbf[:, 128:256]
        mask_ap = cbf[:, 256:768]
        def m_ap(kind, g):
            o = 768 + (kind * 4 + g) * 128
            return cbf[:, o:o + 128]
        g128_ap = cf32[:, 0:512]
        vsc_ap = cf32[:, 512:1024]
        epsx_ap = cf32[:, 1024:1536]
        def gcol(idx, k):
            o = idx * 8 + k
            return smallp[:, o:o + 1]
        def psc(l, g):
            o = (DEPTH * 3 + 1) * 8 + l * 4 + g
            return smallp[:, o:o + 1]
        o_misc = (DEPTH * 3 + 1) * 8 + DEPTH * 4
        inv_ap = smallp[:, o_misc:o_misc + 1]
        phc_ap = smallp[:, o_misc + 1:o_misc + 2]
        phs_ap = smallp[:, o_misc + 2:o_misc + 3]
        C2 = [Buf(C2t[:, s * ST:(s + 1) * ST]) for s in range(NS)]
        SS = [Buf(SSt[:, s * ST:(s + 1) * ST]) for s in range(NS)]
        POSb = Buf(POSI[:])
        WPP = Buf(wpp_t[:])
        RSTD = [Buf(RSTDt[:, i, :]) for i in range(2)]
        KIb = Buf(KI[:])
        U = [Buf(Ut[:, l, :]) for l in range(L)]
        SBF = [Buf(Sbf_t[:, i, :]) for i in range(2)]
        R = [Buf(Rt[:, c, :]) for c in range(NCT)]
        RC = [Buf(Rc[:, l, :]) for l in range(L)]
        TP = [Buf(TPt[:, i, :]) for i in range(NTP)]
        BT = [Buf(BTt[:, i, :]) for i in range(NBT)]
        PB = [Buf(PSt[i][:]) for i in range(7)]
        PBB1 = Buf(PSB[:, 0, :])
        rot = {"tp": 0, "bt": 0, "pb": 0, "pbb": 0, "rs": 0}

        def tmp():
            rot["tp"] = (rot["tp"] + 1) % NTP
            return TP[rot["tp"]]

        def btmp():
            rot["bt"] = (rot["bt"] + 1) % NBT
            return BT[rot["bt"]]

        def bank():
            rot["pb"] = (rot["pb"] + 1) % 7
            return PB[rot["pb"]]

        def bbank():
            rot["pbb"] = (rot["pbb"] + 1) % 2
            return PBB[rot["pbb"]]

        loads = []
        for tt in range(n_tiles):
            for l in range(L):
                for g in range(5):
                    loads.append((w_in_d[l, g], SLOT))
                for g in range(2):
                    loads.append((w_out_d[l, g], SLOT))
                for g in range(11):
                    loads.append((w_gu_d[l, g], SLOT))
                for g in range(8):
                    loads.append((w_dn_d[l, g], FF))
                for g in range(2):
                    loads.append((w_pg_d[l, g], SLOT))
        wstate = {"issued": 0, "acq": 0, "rel": set(), "upto": 0}

        def try_issue():
            while wstate["issued"] < len(loads) and wstate["issued"] < wstate["upto"] + RING:
                i = wstate["issued"]
                src, n = loads[i]
                slot = ring[i % RING]
                dst = slot.ap[:, 0:n]
                S.dma("pool", "w%d" % (i % RING), lambda e, d=dst, s_=src: e.dma_start(out=d, in_=s_), writes=[slot])
                wstate["issued"] += 1

        def acquire():
            i = wstate["acq"]
            wstate["acq"] += 1
            assert i < wstate["upto"] + RING
            try_issue()
            assert i < wstate["issued"]
            return i, ring[i % RING]

        def release(i):
            wstate["rel"].add(i)
            while wstate["upto"] in wstate["rel"]:
                wstate["upto"] += 1
            try_issue()

        S.dma("sp", "c0", lambda e: e.dma_start(out=cbf[:], in_=cbf_d), writes=[CB])
        S.dma("sp", "c1", lambda e: e.dma_start(out=cf32[:], in_=cf32_d), writes=[CF])
        S.dma("sp", "c2", lambda e: e.dma_start(out=smallp[:], in_=smallp_d), writes=[SP_])
        for l in range(L):
            S.op("pool", lambda e, l=l: e.memset(Ut[:, l, :], 0.0), writes=[U[l]])
        try_issue()

        def mm(out_ap, lhsT_ap, rhs_ap, start, stop):
            return lambda e: e.matmul(out_ap, lhsT_ap, rhs_ap, start=start, stop=stop)

        def norm(s, gidx, dst, final_out=None):
            ssb = bank()
            for k in range(8):
                sq = btmp()
                S.op("act", lambda e, o=sq.ap, i=X[k][s].ap: e.activation(o, i, AF.Square),
                     reads=[X[k][s]], writes=[sq])
                S.mm_group([mm(ssb.ap, ones_ap, sq.ap, k == 0, k == 7)], reads=[sq, CB], writes=[ssb] if k == 0 else [])
                if k > 0:
                    ssb.w["pe"] = S.cnt["pe"]
            t = tmp()
            S.op("act", lambda e, o=t.ap, i=ssb.ap: e.activation(o, i, AF.Ln, bias=EPS, scale=1.0 / D),
                 reads=[ssb], writes=[t])
            rot["rs"] = (rot["rs"] + 1) % 2
            r = RSTD[rot["rs"]]
            S.op("act", lambda e, o=r.ap, i=t.ap: e.activation(o, i, AF.Exp, scale=-0.5),
                 reads=[t], writes=[r])
            for k in range(8):
                if final_out is None:
                    S.op("dve", lambda e, o=dst[k].ap, i=X[k][s].ap, g=gcol(gidx, k), rr=r.ap:
                         e.scalar_tensor_tensor(o, i, g, rr, ALU.mult, ALU.mult),
                         reads=[X[k][s], r, SP_], writes=[dst[k]])
                else:
                    ob = tmp()
                    S.op("dve", lambda e, o=ob.ap, i=X[k][s].ap, g=gcol(gidx, k), rr=r.ap:
                         e.scalar_tensor_tensor(o, i, g, rr, ALU.mult, ALU.mult),
                         reads=[X[k][s], r, SP_], writes=[ob])
                    final_out(k, ob)

        def proj_group(slot, lhs_of_k, rhs_bufs, nk):
            b = bank()
            fns = [mm(b.ap, lhs_of_k(k), rhs_bufs[k].ap, k == 0, k == nk - 1) for k in range(nk)]
            S.mm_group(fns, reads=[slot] + list(rhs_bufs), writes=[b])
            return b

        for tt in range(n_tiles):
            t0 = tt * T
            def load_x(tt_, s_):
                t0_ = tt_ * T
                for half in range(2):
                    ks = range(4 * half, 4 * half + 4)
                    bufs = [X[k][s_] for k in ks]
                    S.dma("sp", "x%d" % (s_ * 2 + half),
                          lambda e, half=half, s_=s_, t0_=t0_: e.dma_start(
                              out=Xt[:, 4 * half:4 * half + 4, s_ * ST:(s_ + 1) * ST],
                              in_=xT[4 * half:4 * half + 4, :, t0_ + s_ * ST:t0_ + (s_ + 1) * ST].rearrange("k p t -> p k t")),
                          writes=bufs)

            def build_tables(tt_):
                t0_ = tt_ * T
                for s in range(NS):
                    S.dma("sp", "pos", lambda e, s=s, t0_=t0_: e.dma_start(out=POSI[:], in_=posr[:, t0_ + s * ST:t0_ + (s + 1) * ST]), writes=[POSb])
                    a0 = tmp()
                    S.op("dve", lambda e, o=a0.ap: e.tensor_copy(o, POSI[:]), reads=[POSb], writes=[a0])
                    for (tab, ph) in ((C2[s], phc_ap), (SS[s], phs_ap)):
                        a1 = tmp()
                        S.op("dve", lambda e, o=a1.ap, i=a0.ap, ph=ph: e.tensor_scalar(o, i, inv_ap, ph, ALU.mult, ALU.add),
                             reads=[a0, SP_], writes=[a1])
                        kf = tmp()
                        S.op("dve", lambda e, o=kf.ap, i=a1.ap: e.tensor_scalar(o, i, 1.0 / TWO_PI, None, ALU.mult),
                             reads=[a1], writes=[kf])
                        S.op("dve", lambda e, i=kf.ap: e.tensor_copy(KI[:], i), reads=[kf], writes=[KIb])
                        kf2 = tmp()
                        S.op("dve", lambda e, o=kf2.ap: e.tensor_copy(o, KI[:]), reads=[KIb], writes=[kf2])
                        a2 = tmp()
                        S.op("dve", lambda e, o=a2.ap, k_=kf2.ap, a=a1.ap: e.scalar_tensor_tensor(o, k_, -CW1, a, ALU.mult, ALU.add),
                             reads=[kf2, a1], writes=[a2])
                        a3 = tmp()
                        S.op("dve", lambda e, o=a3.ap, k_=kf2.ap, a=a2.ap: e.scalar_tensor_tensor(o, k_, -CW2, a, ALU.mult, ALU.add),
                             reads=[kf2, a2], writes=[a3])
                        S.op("act", lambda e, o=tab.ap, i=a3.ap: e.activation(o, i, AF.Sin), reads=[a3], writes=[tab])

            if tt == 0:
                load_x(0, 0)
                load_x(0, 1)
                build_tables(0)
            has_next = (tt + 1 < n_tiles)

            for l in range(L):
                first_chunk_global = (tt == 0)
                S.barrier(["dve", "act", "pool"])
                S.dma("pool", "pt", lambda e, l=l, t0=t0: e.dma_start(out=PTt[:], in_=pT[l, :, :, t0:t0 + T].rearrange("k p t -> p k t")),
                      writes=PTall)
                S.dma("pool", "pw", lambda e, l=l: e.dma_start(out=poolw_t[:], in_=w_pool_d[l]), writes=[POOLW])
                S.dma("pool", "pp", lambda e, l=l: e.dma_start(out=wpp_t[:], in_=w_pp_d[l]), writes=[WPP])

                if l == 0:
                    norm(0, l * 3 + 0, [Hb[k][0] for k in range(8)])
                norm(1, l * 3 + 0, [Hb[k][1] for k in range(8)])
                for gi, dstT in ((0, QT), (1, KT)):
                    wi, slot = acquire()
                    for s in range(NS):
                        hs = [Hb[k][s] for k in range(8)]
                        for j in range(4):
                            b = proj_group(slot, lambda k, j=j, slot=slot: slot.ap[:, (j * 8 + k) * 128:(j * 8 + k + 1) * 128], hs, 8)
                            t1 = tmp()
                            S.op("dve", lambda e, o=t1.ap, i=b.ap, c=C2[s].ap: e.tensor_tensor(o, i, c, ALU.mult),
                                 reads=[b, C2[s]], writes=[t1])
                            t2 = tmp()
                            S.op("dve", lambda e, o=t2.ap, i=b.ap, c=SS[s].ap: e.tensor_tensor(o[0:64, :], i[64:128, :], c[64:128, :], ALU.mult),
                                 reads=[b, SS[s]], writes=[t2])
                            S.op("dve", lambda e, o=t2.ap, i=b.ap, c=SS[s].ap: e.tensor_tensor(o[64:128, :], i[0:64, :], c[0:64, :], ALU.mult),
                                 reads=[b, SS[s]], writes=[t2])
                            S.op("pool", lambda e, o=dstT[j][s].ap, a=t1.ap, bb=t2.ap: e.tensor_tensor(o, a, bb, ALU.add),
                                 reads=[t1, t2], writes=[dstT[j][s]])
                    release(wi)
                wi, slot = acquire()
                for s in range(NS):
                    for c in range(4):
                        hs = [Hb[k][s] for k in range(8)]
                        b = bank()
                        fns = [mm(b.ap, Hb[k][s].ap[:, c * 128:(c + 1) * 128], slot.ap[:, k * 512:(k + 1) * 512], k == 0, k == 7)
                               for k in range(8)]
                        S.mm_group(fns, reads=[slot] + hs, writes=[b])
                        vb = Vb[s * 4 + c]
                        S.op("dve", lambda e, o=vb.ap, i=b.ap: e.tensor_tensor(o, i, vsc_ap, ALU.mult),
                             reads=[b, CF], writes=[vb])
                release(wi)
                wi, slot = acquire()
                for s in range(NS):
                    hs = [Hb[k][s] for k in range(8)]
                    for j in range(4):
                        b = proj_group(slot, lambda k, j=j, slot=slot: slot.ap[:, (j * 8 + k) * 128:(j * 8 + k + 1) * 128], hs, 8)
                        o_ap = SG[s].ap.rearrange("p (c h q) -> p c h q", c=4, h=4)[:, :, j, :]
                        i_ap = b.ap.rearrange("p (c q) -> p c q", c=4)
                        S.op("act", lambda e, o=o_ap, i=i_ap: e.activation(o, i, AF.Silu), reads=[b], writes=[SG[s]])
                release(wi)
                wi, slot = acquire()
                for s in range(NS):
                    hs = [Hb[k][s] for k in range(8)]
                    for j in range(4):
                        b = proj_group(slot, lambda k, j=j, slot=slot: slot.ap[:, (j * 8 + k) * 128:(j * 8 + k + 1) * 128], hs, 8)
                        S.op("act", lambda e, o=UT[j][s].ap, i=b.ap: e.activation(o, i, AF.Copy), reads=[b], writes=[UT[j][s]])
                release(wi)

                sbi = 0
                S.op("dve", lambda e, u=U[l].ap: e.tensor_tensor(Sbf_t[:, 0, :], u, g128_ap, ALU.mult),
                     reads=[U[l], CF], writes=[SBF[0]])
                for s in range(NS):
                    mixr = [Hb[k][s] for k in range(4)]

                    def emit_scores(c):
                        cs = slice(c * 128, (c + 1) * 128)
                        sb_ = bank()
                        S.mm_group([mm(sb_.ap[:, h * 128:(h + 1) * 128], KT[h][s].ap[:, cs], QT[h][s].ap[:, cs], True, True)
                                    for h in range(4)], reads=[KT[h][s] for h in range(4)] + [QT[h][s] for h in range(4)], writes=[sb_])
                        pm = btmp()
                        S.op("dve", lambda e, o=pm.ap, i=sb_.ap: e.tensor_tensor(o, i, mask_ap, ALU.mult),
                             reads=[sb_, CB], writes=[pm])
                        return pm

                    def emit_transposes(c):
                        cs = slice(c * 128, (c + 1) * 128)
                        tb = PBB1
                        S.mm_group([lambda e, h=h, o=tb.ap, i=KT[h][s].ap[:, cs]: e.transpose(o[:, h * 128:(h + 1) * 128], i, ident_ap)
                                    for h in range(4)], reads=[KT[h][s] for h in range(4)] + [CB], writes=[tb])
                        ktok = btmp()
                        S.op("act", lambda e, o=ktok.ap, i=tb.ap: e.activation(o, i, AF.Copy), reads=[tb], writes=[ktok])
                        return ktok

                    for c in range(4):
                        cs = slice(c * 128, (c + 1) * 128)
                        rb = bank()
                        S.mm_group([mm(rb.ap[:, g * 128:(g + 1) * 128], UT[g][s].ap[:, cs], poolw_t[:, g * 128:(g + 1) * 128], True, True)
                                    for g in range(4)], reads=[UT[g][s] for g in range(4)] + [POOLW], writes=[rb])
                        rr = R[s * 4 + c]
                        S.op("act", lambda e, o=rr.ap, i=rb.ap: e.activation(o, i, AF.Copy), reads=[rb], writes=[rr])

                    def emit_pool2(g):
                        gc = slice(g * 128, (g + 1) * 128)
                        yb2 = bank()
                        fns = []
                        rds = [CB]
                        for c2 in range(4):
                            cs2 = slice(c2 * 128, (c2 + 1) * 128)
                            n = s * 4 + c2
                            cur = R[n]
                            rds.append(cur)
                            if first_chunk_global and n == 0:
                                fns.append(mm(yb2.ap[:, cs2], cur.ap[:, gc], m_ap(2, g), True, True))
                            else:
                                prev = R[n - 1] if n > 0 else RC[l]
                                rds.append(prev)
                                fns.append(mm(yb2.ap[:, cs2], cur.ap[:, gc], m_ap(0, g), True, False))
                                fns.append(mm(yb2.ap[:, cs2], prev.ap[:, gc], m_ap(1, g), False, True))
                        S.mm_group(fns, reads=rds, writes=[yb2])
                        S.op("act", lambda e, o=Hb[4 + g][s].ap, i=yb2.ap, sc=psc(l, g): e.activation(o, i, AF.Copy, scale=sc),
                             reads=[yb2, SP_], writes=[Hb[4 + g][s]])

                    pm_n = emit_scores(0)
                    ktok_n = emit_transposes(0)
                    for c in range(4):
                        cs = slice(c * 128, (c + 1) * 128)
                        vb = Vb[s * 4 + c]
                        pm, ktok = pm_n, ktok_n
                        sbf_cur = SBF[sbi]
                        sbf_ap = Sbf_t[:, sbi, :]
                        kvb = bank()
                        S.mm_group([mm(kvb.ap[:, h * 128:(h + 1) * 128], ktok.ap[:, h * 128:(h + 1) * 128], vb.ap[:, h * 128:(h + 1) * 128], True, True)
                                    for h in range(4)], reads=[ktok, vb], writes=[kvb])
                        S.group("dve", [lambda e, u=U[l].ap, kv=kvb.ap, hc=slice(h * 128, (h + 1) * 128), g=float(GAMMA[h] ** 128):
                                        e.scalar_tensor_tensor(u[:, hc], u[:, hc], g, kv[:, hc], ALU.mult, ALU.add) for h in range(4)],
                                reads=[kvb], writes=[U[l]])
                        last = (s == NS - 1 and c == 3)
                        if not last:
                            nxt = 1 - sbi
                            S.op("dve", lambda e, u=U[l].ap, nxt=nxt: e.tensor_tensor(Sbf_t[:, nxt, :], u, g128_ap, ALU.mult),
                                 reads=[U[l], CF], writes=[SBF[nxt]])
                        yb = bank()
                        fns = []
                        for h in range(4):
                            hc = slice(h * 128, (h + 1) * 128)
                            fns.append(mm(yb.ap[:, hc], vb.ap[:, hc], pm.ap[:, hc], True, False))
                            fns.append(mm(yb.ap[:, hc], sbf_ap[:, hc], QT[h][s].ap[:, cs], False, True))
                        S.mm_group(fns, reads=[vb, pm, sbf_cur] + [QT[h][s] for h in range(4)], writes=[yb])
                        sbi = 1 - sbi
                        emit_pool2(c)
                        if c < 3:
                            pm_n = emit_scores(c + 1)
                            ktok_n = emit_transposes(c + 1)
                        ysq = btmp()
                        S.op("act", lambda e, o=ysq.ap, i=yb.ap: e.activation(o, i, AF.Square), reads=[yb], writes=[ysq])
                        gsb = bank()
                        S.mm_group([mm(gsb.ap, ones_ap, ysq.ap, True, True)], reads=[ysq, CB], writes=[gsb])
                        tg = tmp()
                        S.op("dve", lambda e, o=tg.ap, i=gsb.ap: e.scalar_tensor_tensor(o, i, 1.0 / 128.0, epsx_ap, ALU.mult, ALU.add),
                             reads=[gsb, CF], writes=[tg])
                        rs = tmp()
                        S.op("act", lambda e, o=rs.ap, i=tg.ap: e.activation(o, i, AF.Ln), reads=[tg], writes=[rs])
                        S.op("act", lambda e, o=rs.ap: e.activation(o, o, AF.Exp, scale=-0.5), reads=[rs], writes=[rs])
                        rs2 = tmp()
                        S.op("pool", lambda e, o=rs2.ap, i=rs.ap, g=SG[s].ap[:, c * 512:(c + 1) * 512]: e.tensor_tensor(o, i, g, ALU.mult),
                             reads=[rs, SG[s]], writes=[rs2])
                        o_ap = Ht[:, 0:4, s * ST + c * 128: s * ST + (c + 1) * 128]
                        S.op("dve", lambda e, o=o_ap, y=yb.ap.rearrange("p (h q) -> p h q", h=4), r_=rs2.ap.rearrange("p (h q) -> p h q", h=4):
                             e.tensor_tensor(o, y, r_, ALU.mult), reads=[yb, rs2], writes=mixr)
                S.op("pool", lambda e, l=l: e.tensor_copy(Rc[:, l, :], Rt[:, NCT - 1, :]), reads=[R[NCT - 1]], writes=[RC[l]])

                if dbg and l == 0 and tt == 0:
                    allh = [Hb[k][s] for k in range(8) for s in range(NS)]
                    S.dma("sp", "dbg", lambda e: e.dma_start(out=dbgH, in_=Ht[:].rearrange("p k t -> p (k t)")), reads=allh)
                    allqk = [QT[h][s] for h in range(4) for s in range(NS)] + [KT[h][s] for h in range(4) for s in range(NS)]
                    S.dma("sp", "dbg", lambda e: e.dma_start(out=dbgQK, in_=REG[:, 0:8 * T]), reads=allqk)
                wslots = [acquire(), acquire()]

                def wout_block(s, jd):
                    wi_, slot = wslots[jd // 4]
                    j = jd % 4
                    hs = [Hb[k][s] for k in range(8)]
                    b = proj_group(slot, lambda k, j=j, slot=slot: slot.ap[:, (j * 8 + k) * 128:(j * 8 + k + 1) * 128], hs, 8)
                    S.op("dve", lambda e, x=X[jd][s].ap, i=b.ap: e.tensor_tensor(x, i, x, ALU.add),
                         reads=[b], writes=[X[jd][s]])

                for jd in range(8):
                    wout_block(0, jd)
                for jd in range(2):
                    wout_block(1, jd)
                norm(0, l * 3 + 1, [Hb[k][0] for k in range(8)])
                for jd in range(2, 8):
                    wout_block(1, jd)
                release(wslots[0][0])
                release(wslots[1][0])

                norm(1, l * 3 + 1, [Hb[k][1] for k in range(8)])
                S.barrier(["dve"])
                def gu_blocks(gi, slot, s):
                    hs = [Hb[k][s] for k in range(8)]
                    for jj in range(2):
                        f = gi * 2 + jj
                        base = jj * 2 * 8 * 128
                        ba = proj_group(slot, lambda k, base=base, slot=slot: slot.ap[:, base + k * 128: base + (k + 1) * 128], hs, 8)
                        bu = proj_group(slot, lambda k, base=base, slot=slot: slot.ap[:, base + (8 + k) * 128: base + (9 + k) * 128], hs, 8)
                        sg = tmp()
                        S.op("act", lambda e, o=sg.ap, i=ba.ap: e.activation(o, i, AF.Silu), reads=[ba], writes=[sg])
                        S.op("dve", lambda e, o=ACTT[f][s].ap, i=bu.ap, g=sg.ap: e.tensor_tensor(o, i, g, ALU.mult),
                             reads=[bu, sg], writes=[ACTT[f][s]])

                gi = 0
                while gi < 11:
                    grp = [gi] if gi == 10 else [gi, gi + 1]
                    held = [(g_, acquire()) for g_ in grp]
                    for s in range(NS):
                        for g_, (wi_, slot) in held:
                            gu_blocks(g_, slot, s)
                    for g_, (wi_, slot) in held:
                        release(wi_)
                    gi += len(grp)
                if l == L - 1 and has_next:
                    build_tables(tt + 1)
                def down_block(jd, slot, s):
                    acts = [ACTT[f][s] for f in range(NFB)]
                    b = proj_group(slot, lambda k, slot=slot: slot.ap[:, k * 128:(k + 1) * 128], acts, NFB)
                    S.op("dve", lambda e, x=X[jd][s].ap, i=b.ap: e.tensor_tensor(x, i, x, ALU.add),
                         reads=[b], writes=[X[jd][s]])

                for jd in range(5):
                    wi, slot = acquire()
                    for s in range(NS):
                        down_block(jd, slot, s)
                    release(wi)
                held = [(jd, acquire()) for jd in (5, 6, 7)]
                for jd, (wi_, slot) in held:
                    down_block(jd, slot, 0)
                down_block(5, held[0][1][1], 1)
                norm(0, l * 3 + 2, [Hb[k][0] for k in range(8)])
                for jd, (wi_, slot) in held[1:]:
                    down_block(jd, slot, 1)
                for jd, (wi_, slot) in held:
                    release(wi_)

                norm(1, l * 3 + 2, [Hb[k][1] for k in range(8)])
                wpp = WPP
                pslots = [acquire(), acquire()]

                def ple_block(s, jd):
                    wi_, slot = pslots[jd // 4]
                    j = jd % 4
                    hs = [Hb[k][s] for k in range(8)]
                    ba = proj_group(slot, lambda k, j=j, slot=slot: slot.ap[:, (j * 8 + k) * 128:(j * 8 + k + 1) * 128], hs, 8)
                    be = proj_group(wpp, lambda k, jd=jd, wpp=wpp: wpp.ap[:, (jd * 2 + k) * 128:(jd * 2 + k + 1) * 128],
                                    [PT[0][s], PT[1][s]], 2)
                    th = tmp()
                    S.op("act", lambda e, o=th.ap, i=ba.ap: e.activation(o, i, AF.Tanh, scale=0.5), reads=[ba], writes=[th])
                    tm = tmp()
                    S.op("dve", lambda e, o=tm.ap, t_=th.ap, i=be.ap: e.scalar_tensor_tensor(o, t_, 1.0, i, ALU.add, ALU.mult),
                         reads=[th, be], writes=[tm])
                    S.op("dve", lambda e, x=X[jd][s].ap, t_=tm.ap: e.scalar_tensor_tensor(x, t_, 0.5, x, ALU.mult, ALU.add),
                         reads=[tm], writes=[X[jd][s]])

                def store_fn(s, t0=t0):
                    def store(k, ob):
                        S.dma("sp", "o%d" % (k % 4),
                              lambda e, k=k, ob=ob, s=s, t0=t0: e.dma_start(out=outT[k, :, t0 + s * ST: t0 + (s + 1) * ST], in_=ob.ap),
                              reads=[ob])
                    return store

                for jd in range(8):
                    ple_block(0, jd)
                for jd in range(2):
                    ple_block(1, jd)
                if l < L - 1:
                    norm(0, (l + 1) * 3 + 0, [Hb[k][0] for k in range(8)])
                else:
                    norm(0, DEPTH * 3, None, final_out=store_fn(0))
                    if has_next:
                        load_x(tt + 1, 0)
                for jd in range(2, 8):
                    ple_block(1, jd)
                release(pslots[0][0])
                release(pslots[1][0])

            norm(1, DEPTH * 3, None, final_out=store_fn(1))
            if has_next:
                load_x(tt + 1, 1)

        S.final_wait("sp")

        sem_names = ["pe", "dve", "act", "pool"] + sorted(S.dma_cnt.keys())
        sems = {n: es.enter_context(nc.semaphore("s_" + n.replace(":", "_"))) for n in sem_names}
        block = es.enter_context(nc.Block())

        def replay(stream, ename):
            def run(eng):
                for item in stream:
                    if item[0] == "wait":
                        eng.wait_ge(sems[item[1]], item[2])
                    elif item[0] == "op":
                        ins = item[1](eng)
                        if item[2]:
                            ins.then_inc(sems[ename], 1)
                    else:
                        item[1](eng).then_inc(sems[item[2]], 16)
            return run

        block.tensor(replay(S.streams["pe"], "pe"))
        block.vector(replay(S.streams["dve"], "dve"))
        block.scalar(replay(S.streams["act"], "act"))
        block.gpsimd(replay(S.streams["pool"], "pool"))
        block.sync(replay(S.streams["sp"], "sp"))
    print("instr counts:", {e: len(v) for e, v in S.streams.items()})
    return nc


def _blk(w, ncol_blocks):
    K = w.shape[0] // 128
    return w.reshape(K, 128, ncol_blocks, 128).transpose(2, 1, 0, 3)


def _const_tables():
    bf = ml_dtypes.bfloat16
    ones = np.ones((128, 128), np.float32)
    ident = np.eye(128, dtype=np.float32)
    k = np.arange(128)[:, None]
    q = np.arange(128)[None, :]
    mask = (k <= q).astype(np.float32)
    mask4 = np.tile(mask, (1, 4))
    ms = []
    tp = np.arange(128)[:, None]
    t = np.arange(128)[None, :]
    for w in WINDOWS:
        m = ((t - tp >= 0) & (t - tp < w)).astype(np.float32) / w - (t == tp).astype(np.float32)
        ms.append(m)
    for w in WINDOWS:
        m = ((t + 128 - tp) < w).astype(np.float32) / w
        ms.append(m)
    for w in WINDOWS:
        cnt = np.minimum(t + 1, w).astype(np.float32)
        m = ((t - tp >= 0) & (t - tp < w)).astype(np.float32) / cnt - (t == tp).astype(np.float32)
        ms.append(m)
    cbf = np.concatenate([ones, ident, mask4] + ms, axis=1).astype(bf)
    g128 = np.zeros((128, 512), np.float32)
    vsc = np.zeros((128, 512), np.float32)
    epsx = np.zeros((128, 512), np.float32)
    p = np.arange(128, dtype=np.float64)
    for h in range(NH):
        g = GAMMA[h]
        g128[:, h * 128:(h + 1) * 128] = g ** 128
        vsc[:, h * 128:(h + 1) * 128] = (g ** (-(p + 1.0)))[:, None]
        epsx[:, h * 128:(h + 1) * 128] = (128.0 * EPS * g ** (-2.0 * (p + 1.0)))[None, :]
    cf32 = np.concatenate([g128, vsc, epsx], axis=1).astype(np.float32)
    return np.ascontiguousarray(cbf), np.ascontiguousarray(cf32)


def _prep_shared(g_mix, w_in, pool_w, pool_scale, w_out, g_ffn, w_gate, w_up, w_down, g_ple,
                 w_ple_gate, w_ple_proj, g_final):
    f = np.float32
    w_in_r = np.empty((DEPTH, 5, 128, SLOT), f)
    w_out_r = np.empty((DEPTH, 2, 128, SLOT), f)
    w_gu_r = np.empty((DEPTH, 11, 128, SLOT), f)
    w_dn_r = np.empty((DEPTH, 8, 128, FF), f)
    w_pg_r = np.empty((DEPTH, 2, 128, SLOT), f)
    w_pp_r = np.empty((DEPTH, 128, 2048), f)
    w_pool_r = np.empty((DEPTH, 128, 512), f)
    for l in range(DEPTH):
        wb = _blk(w_in[l], 20)
        for gi, gsrc in ((0, 0), (1, 1), (3, 3), (4, 4)):
            w_in_r[l, gi] = wb[gsrc * 4:(gsrc + 1) * 4].transpose(1, 0, 2, 3).reshape(128, SLOT)
        wv = w_in[l][:, 1024:1536].reshape(8, 128, 512).transpose(1, 0, 2)
        w_in_r[l, 2] = wv.reshape(128, SLOT)
        wo = _blk(w_out[l], 8)
        wp = _blk(w_ple_gate[l], 8)
        for gi in range(2):
            w_out_r[l, gi] = wo[gi * 4:(gi + 1) * 4].transpose(1, 0, 2, 3).reshape(128, SLOT)
            w_pg_r[l, gi] = wp[gi * 4:(gi + 1) * 4].transpose(1, 0, 2, 3).reshape(128, SLOT)
        wg = _blk(w_gate[l], NFB)
        wu = _blk(w_up[l], NFB)
        gu = np.stack([wg, wu], axis=1)
        gu = gu.reshape(11, 2, 2, 128, 8, 128).transpose(0, 3, 1, 2, 4, 5)
        w_gu_r[l] = gu.reshape(11, 128, SLOT)
        wd = _blk(w_down[l], 8)
        w_dn_r[l] = wd.reshape(8, 128, FF)
        pp = _blk(w_ple_proj[l], 8)
        w_pp_r[l] = pp.transpose(1, 0, 2, 3).reshape(128, 2048)
        w_pool_r[l] = pool_w[l].transpose(1, 0, 2).reshape(128, 512)
    NSP = (DEPTH * 3 + 1) * 8 + DEPTH * 4 + 4
    smallp = np.zeros((128, NSP), f)
    for l in range(DEPTH):
        for i, g in enumerate((g_mix[l], g_ffn[l], g_ple[l])):
            idx = l * 3 + i
            smallp[:, idx * 8:(idx + 1) * 8] = g.reshape(8, 128).T
        smallp[:, (DEPTH * 3 + 1) * 8 + l * 4:(DEPTH * 3 + 1) * 8 + (l + 1) * 4] = pool_scale[l].reshape(4, 128).T
    smallp[:, DEPTH * 3 * 8:(DEPTH * 3 + 1) * 8] = g_final.reshape(8, 128).T
    o = (DEPTH * 3 + 1) * 8 + DEPTH * 4
    half = 64
    inv = (np.float32(1.0) / (np.float32(10000.0) ** (np.arange(half, dtype=np.float32) * np.float32(2.0 / 128)))).astype(f)
    smallp[:, o] = np.concatenate([inv, inv])
    smallp[:, o + 1] = np.float32(np.pi / 2)
    smallp[:, o + 2] = np.concatenate([np.zeros(64, f), np.full(64, np.pi, f)])
    cbf, cf32 = _const_tables()
    return dict(w_in_r=w_in_r, w_out_r=w_out_r, w_gu_r=w_gu_r, w_dn_r=w_dn_r, w_pg_r=w_pg_r, w_pp_r=w_pp_r,
                w_pool_r=w_pool_r, smallp=smallp, cbf=cbf, cf32=cf32)


def run(inputs, n_layers=DEPTH, n_tiles=SEQ // T, cores=8, dbg=False):
    x = np.asarray(inputs["x"], np.float32)
    p = np.asarray(inputs["p"], np.float32)
    positions = np.asarray(inputs["positions"], np.int32)
    shared = _prep_shared(*[np.asarray(inputs[k], np.float32) for k in (
        "g_mix", "w_in", "pool_w", "pool_scale", "w_out", "g_ffn", "w_gate", "w_up", "w_down", "g_ple",
        "w_ple_gate", "w_ple_proj", "g_final")])
    nc = build_program(n_layers, n_tiles, dbg)
    in_maps = []
    for b in range(cores):
        m = dict(shared)
        m["xT"] = np.ascontiguousarray(x[b].T).reshape(8, 128, SEQ)
        m["pT"] = np.ascontiguousarray(p[:, b].transpose(0, 2, 1)).reshape(DEPTH, 2, 128, SEQ)
        m["posr"] = np.ascontiguousarray(np.broadcast_to(positions[b][None, :], (128, SEQ)))
        in_maps.append(m)
    res = run_bass_kernel_spmd(nc, in_maps, core_ids=list(range(cores)))
    outs = [np.ascontiguousarray(r["outT"].reshape(D, SEQ).T) for r in res.results]
    if dbg:
        return np.stack(outs, axis=0), res.results[0]
    return np.stack(outs, axis=0)


def kernel(**inputs):
    return run(inputs).astype(np.float32)
```
